# Optimizing a Trainium2 kernel written in Bass

```python
import math
import jax, jax.numpy as jnp
from jax import lax
import numpy as np

D_MODEL = 2048
BATCH = 2
SEQ = 4096
DEPTH = 2

HEAD_DIM = 64
A_Q_HEADS = 16
A_KV_HEADS = 2
A_GROUP = A_Q_HEADS // A_KV_HEADS
A_WINDOW = 128
A_BLOCK = 128
T5_BUCKETS = 32
T5_MAX_DIST = 128
B_HEADS = 16
GRID_W = 64
NA_WIN_H = 8
NA_WIN_W = 16
A_WIDTH = A_Q_HEADS * HEAD_DIM
A_KV_WIDTH = A_KV_HEADS * HEAD_DIM
B_WIDTH = B_HEADS * HEAD_DIM
ATTN_IN = A_WIDTH + 2 * A_KV_WIDTH + 3 * B_WIDTH
MIX_WIDTH = A_WIDTH + B_WIDTH
HYENA_ORDER = 2
HYENA_WIDTH = D_MODEL
HYENA_EMB = 33
HYENA_FILTER_HIDDEN = 64
HYENA_SHORT = 3
HYENA_DECAY_TARGET = 1e-2
HYENA_FAST_PCT = 0.3
HYENA_SLOW_PCT = 1.5
D_FF = 5632
N_EXPERTS = 8
TOP_K = 2
D_FF_EXPERT = 7168
MOE_BLOCK = 128
PLE_DIM = 256
RMS_EPS = 1e-6

kernel_name = 'hybrid_swa_natten_hyena_moe_encoder'

F32 = jnp.float32
NEG_INF = -1e30


def rmsnorm(x, g):
    xf = x.astype(F32)
    y = xf * lax.rsqrt(jnp.mean(xf * xf, axis=-1, keepdims=True) + RMS_EPS)
    return (y * g.astype(F32)).astype(x.dtype)


def t5_bucket(rel):
    half = T5_BUCKETS // 2
    max_exact = half // 2
    n = jnp.abs(rel)
    log_ratio = jnp.log(jnp.maximum(n, 1).astype(F32) / max_exact) / math.log(T5_MAX_DIST / max_exact)
    large = jnp.minimum(max_exact + (log_ratio * (half - max_exact)).astype(jnp.int32), half - 1)
    return jnp.where(rel > 0, half, 0) + jnp.where(n < max_exact, n, large)


def window_gqa(q, k, v, t5_bias, sink):
    B, S = q.shape[0], q.shape[1]
    nb = S // A_BLOCK
    qb = q.reshape(B, nb, A_BLOCK, A_KV_HEADS, A_GROUP, HEAD_DIM)
    pad = ((0, 0), (A_WINDOW, A_WINDOW), (0, 0), (0, 0))

    def band(t):
        tp = jnp.pad(t, pad).reshape(B, nb + 2, A_BLOCK, A_KV_HEADS, HEAD_DIM)
        return jnp.concatenate([tp[:, :-2], tp[:, 1:-1], tp[:, 2:]], axis=2)

    kb, vb = band(k), band(v)
    s = jnp.einsum('bnikgd,bnjkd->bnkgij', qb, kb).astype(F32) * (HEAD_DIM ** -0.5)
    i = jnp.arange(A_BLOCK)[:, None]
    j = jnp.arange(3 * A_BLOCK)[None, :]
    rel = j - A_WINDOW - i
    bias = t5_bias.astype(F32)[t5_bucket(rel)]
    bias = bias.transpose(2, 0, 1).reshape(A_KV_HEADS, A_GROUP, A_BLOCK, 3 * A_BLOCK)
    kpos = jnp.arange(nb)[:, None, None] * A_BLOCK - A_WINDOW + j[None]
    ok = (jnp.abs(rel) <= A_WINDOW)[None] & (kpos >= 0) & (kpos < S)
    s = jnp.where(ok[None, :, None, None], s + bias, NEG_INF)
    sk = sink.astype(F32).reshape(A_KV_HEADS, A_GROUP)[None, None, :, :, None, None]
    m = jnp.maximum(s.max(axis=-1, keepdims=True), sk)
    pr = jnp.exp(s - m)
    pr = pr / (pr.sum(axis=-1, keepdims=True) + jnp.exp(sk - m))
    o = jnp.einsum('bnkgij,bnjkd->bnikgd', pr.astype(v.dtype), vb)
    return o.reshape(B, S, A_WIDTH)


def neighbourhood_attention(q, k, v, rpb):
    B, S = q.shape[0], q.shape[1]
    rows = S // GRID_W
    kh = min(NA_WIN_H, rows)
    kw = NA_WIN_W
    r = jnp.arange(rows)
    row_idx = jnp.clip(r - kh // 2, 0, rows - kh)[:, None] + jnp.arange(kh)[None]
    c = jnp.arange(GRID_W)
    cs = jnp.clip(c - kw // 2, 0, GRID_W - kw)
    col_ok = (c[None] >= cs[:, None]) & (c[None] < cs[:, None] + kw)
    row_off = row_idx - r[:, None] + NA_WIN_H - 1
    col_off = jnp.clip(c[None] - c[:, None], -(kw - 1), kw - 1) + kw - 1
    bias = rpb.astype(F32)[:, row_off[:, None, :, None], col_off[None, :, None, :]]
    bias = bias.transpose(1, 0, 2, 3, 4)
    q5 = q.reshape(B, rows, GRID_W, B_HEADS, HEAD_DIM)
    kg = k.reshape(B, rows, GRID_W, B_HEADS, HEAD_DIM)[:, row_idx]
    vg = v.reshape(B, rows, GRID_W, B_HEADS, HEAD_DIM)[:, row_idx]
    s = jnp.einsum('brqhd,brikhd->brhqik', q5, kg).astype(F32) * (HEAD_DIM ** -0.5) + bias[None]
    s = jnp.where(col_ok[:, None, :], s, NEG_INF)
    pr = jax.nn.softmax(s.reshape(B, rows, B_HEADS, GRID_W, kh * GRID_W), axis=-1).reshape(s.shape)
    o = jnp.einsum('brhqik,brikhd->brqhd', pr.astype(v.dtype), vg)
    return o.reshape(B, S, B_WIDTH)


def hyena_filters(L, fw1, fb1, fw2, fb2, freq, fw3):
    t = jnp.linspace(0.0, 1.0, L, dtype=F32)[:, None]
    bands = (HYENA_EMB - 1) // 2
    w = (2.0 * math.pi / L) * jnp.arange(L, dtype=F32)[:, None]
    f = jnp.linspace(1e-4, bands - 1, bands, dtype=F32)[None]
    z = jnp.concatenate([t, jnp.cos(f * w), -jnp.sin(f * w)], axis=-1)
    fr = freq.astype(F32)
    hid = jnp.sin(fr * (z @ fw1.astype(F32) + fb1.astype(F32)))
    hid = jnp.sin(fr * (hid @ fw2.astype(F32) + fb2.astype(F32)))
    h = (hid @ fw3.astype(F32)).reshape(L, 2, HYENA_ORDER, HYENA_WIDTH)
    max_decay = math.log(HYENA_DECAY_TARGET) / HYENA_FAST_PCT
    min_decay = math.log(HYENA_DECAY_TARGET) / HYENA_SLOW_PCT
    deltas = jnp.linspace(min_decay, max_decay, HYENA_WIDTH, dtype=F32)
    decay = jnp.exp(-t * jnp.abs(deltas)[None])
    return h * decay[:, None, None, :]


def hyena_mixer(u, w_in, b_in, conv_w, conv_b, fw1, fb1, fw2, fb2, freq, fw3, fbias, w_out):
    B, L, _ = u.shape
    proj = u @ w_in + b_in
    pp = jnp.pad(proj, ((0, 0), (1, 1), (0, 0)))
    sc = pp[:, :-2] * conv_w[0] + pp[:, 1:-1] * conv_w[1] + pp[:, 2:] * conv_w[2] + conv_b
    x1, x2, z = jnp.split(sc, 3, axis=-1)
    h = hyena_filters(L, fw1, fb1, fw2, fb2, freq, fw3)
    two_sided = jnp.concatenate([h[:1, 0] + h[:1, 1], h[1:, 0],
                                 jnp.zeros((1, HYENA_ORDER, HYENA_WIDTH), F32), h[:0:-1, 1]], axis=0)
    hf = jnp.fft.rfft(two_sided, axis=0)
    zf = z.astype(F32)
    for o, gate in enumerate((x1, x2)):
        conv = jnp.fft.irfft(jnp.fft.rfft(zf, n=2 * L, axis=1) * hf[:, o][None], n=2 * L, axis=1)[:, :L]
        zf = gate.astype(F32) * (conv + zf * fbias[o].astype(F32))
    return zf.astype(u.dtype) @ w_out


def swiglu(x, wg, wu, wd):
    return (jax.nn.silu(x @ wg) * (x @ wu)) @ wd


def moe_swiglu(x2, w_router, wg, wu, wd):
    N, D = x2.shape
    logits = (x2 @ w_router).astype(F32)
    top_v, top_i = lax.top_k(logits, TOP_K)
    gates = jax.nn.softmax(top_v, axis=-1)
    e_flat = top_i.reshape(-1)
    tok_flat = jnp.repeat(jnp.arange(N, dtype=jnp.int32), TOP_K)
    g_flat = gates.reshape(-1)
    order = jnp.argsort(e_flat)
    e_s, tok_s, g_s = e_flat[order], tok_flat[order], g_flat[order]
    counts = jnp.bincount(e_flat, length=N_EXPERTS)
    padded = ((counts + MOE_BLOCK - 1) // MOE_BLOCK) * MOE_BLOCK
    pad_end = jnp.cumsum(padded)
    pad_start = pad_end - padded
    start = jnp.cumsum(counts) - counts
    nk = N * TOP_K
    dest = pad_start[e_s] + jnp.arange(nk) - start[e_s]
    P = ((nk + MOE_BLOCK - 1) // MOE_BLOCK) * MOE_BLOCK + N_EXPERTS * MOE_BLOCK
    row_tok = jnp.full((P,), N, jnp.int32).at[dest].set(tok_s)
    row_gate = jnp.zeros((P,), F32).at[dest].set(g_s)
    nblk = P // MOE_BLOCK
    blk_exp = jnp.minimum(jnp.searchsorted(pad_end, jnp.arange(nblk) * MOE_BLOCK, side='right'),
                          N_EXPERTS - 1)
    x_ext = jnp.concatenate([x2, jnp.zeros((1, D), x2.dtype)], axis=0)
    xs = x_ext[row_tok].reshape(nblk, MOE_BLOCK, D)

    def expert_block(args):
        xb, e = args
        return (jax.nn.silu(xb @ wg[e]) * (xb @ wu[e])) @ wd[e]

    ys = lax.map(expert_block, (xs, blk_exp)).reshape(P, D)
    out = jnp.zeros((N + 1, D), ys.dtype).at[row_tok].add(ys * row_gate[:, None].astype(ys.dtype))
    return out[:N]


def setup_inputs(seed: int = 0) -> dict:
    key = jax.random.key(seed)
    ks = iter(jax.random.split(key, 40))

    def nrm(shape, scale):
        return jax.random.normal(next(ks), shape, F32) * scale

    ne = (DEPTH + 1) // 2
    no = DEPTH // 2
    D = D_MODEL
    W = HYENA_WIDTH
    FH = HYENA_FILTER_HIDDEN
    return {
        'x': nrm((BATCH, SEQ, D), 1.0),
        'p': nrm((DEPTH, BATCH, SEQ, PLE_DIM), 1.0),
        'ln_mix': 1.0 + nrm((DEPTH, D), 0.05),
        'ln_ffn': 1.0 + nrm((DEPTH, D), 0.05),
        'ln_ple': 1.0 + nrm((DEPTH, D), 0.05),
        'final_norm': 1.0 + nrm((D,), 0.05),
        't5_bias': nrm((T5_BUCKETS, A_Q_HEADS), 0.5),
        'w_attn_in': nrm((ne, D, ATTN_IN), D ** -0.5),
        'w_attn_out': nrm((ne, MIX_WIDTH, D), MIX_WIDTH ** -0.5),
        'attn_sink': nrm((ne, A_Q_HEADS), 0.5),
        'na_rpb': nrm((ne, B_HEADS, 2 * NA_WIN_H - 1, 2 * NA_WIN_W - 1), 0.5),
        'w_ffn_gate': nrm((ne, D, D_FF), D ** -0.5),
        'w_ffn_up': nrm((ne, D, D_FF), D ** -0.5),
        'w_ffn_down': nrm((ne, D_FF, D), D_FF ** -0.5),
        'w_hy_in': nrm((no, D, 3 * W), D ** -0.5),
        'b_hy_in': nrm((no, 3 * W), 0.02),
        'w_hy_conv': nrm((no, HYENA_SHORT, 3 * W), HYENA_SHORT ** -0.5),
        'b_hy_conv': nrm((no, 3 * W), 0.02),
        'w_hy_f1': nrm((no, HYENA_EMB, FH), HYENA_EMB ** -0.5),
        'b_hy_f1': nrm((no, FH), 0.1),
        'w_hy_f2': nrm((no, FH, FH), FH ** -0.5),
        'b_hy_f2': nrm((no, FH), 0.1),
        'hy_freq': 1.0 + nrm((no, FH), 0.1),
        'w_hy_f3': nrm((no, FH, 2 * HYENA_ORDER * W), 0.05 * FH ** -0.5),
        'hy_bias': nrm((no, HYENA_ORDER, W), 0.5),
        'w_hy_out': nrm((no, W, D), W ** -0.5),
        'w_router': nrm((no, D, N_EXPERTS), D ** -0.5),
        'w_exp_gate': nrm((no, N_EXPERTS, D, D_FF_EXPERT), D ** -0.5),
        'w_exp_up': nrm((no, N_EXPERTS, D, D_FF_EXPERT), D ** -0.5),
        'w_exp_down': nrm((no, N_EXPERTS, D_FF_EXPERT, D), D_FF_EXPERT ** -0.5),
        'w_ple_proj': nrm((DEPTH, PLE_DIM, D), PLE_DIM ** -0.5),
        'w_ple_gate': nrm((DEPTH, D, D), D ** -0.5),
    }


def reference(x, p, ln_mix, ln_ffn, ln_ple, final_norm, t5_bias, w_attn_in, w_attn_out,
              attn_sink, na_rpb, w_ffn_gate, w_ffn_up, w_ffn_down, w_hy_in, b_hy_in,
              w_hy_conv, b_hy_conv, w_hy_f1, b_hy_f1, w_hy_f2, b_hy_f2, hy_freq, w_hy_f3,
              hy_bias, w_hy_out, w_router, w_exp_gate, w_exp_up, w_exp_down,
              w_ple_proj, w_ple_gate):
    B, S, D = x.shape
    o1 = A_WIDTH
    o2 = o1 + A_KV_WIDTH
    o3 = o2 + A_KV_WIDTH
    o4 = o3 + B_WIDTH
    o5 = o4 + B_WIDTH
    h = x
    for i in range(DEPTH):
        li = i // 2
        hn = rmsnorm(h, ln_mix[i])
        if i % 2 == 0:
            proj = hn @ w_attn_in[li]
            qa, ka, va, qn, kn, vn = jnp.split(proj, [o1, o2, o3, o4, o5], axis=-1)
            oa = window_gqa(qa.reshape(B, S, A_Q_HEADS, HEAD_DIM),
                            ka.reshape(B, S, A_KV_HEADS, HEAD_DIM),
                            va.reshape(B, S, A_KV_HEADS, HEAD_DIM), t5_bias, attn_sink[li])
            ob = neighbourhood_attention(qn.reshape(B, S, B_HEADS, HEAD_DIM),
                                         kn.reshape(B, S, B_HEADS, HEAD_DIM),
                                         vn.reshape(B, S, B_HEADS, HEAD_DIM), na_rpb[li])
            h = h + jnp.concatenate([oa, ob], axis=-1) @ w_attn_out[li]
            h = h + swiglu(rmsnorm(h, ln_ffn[i]), w_ffn_gate[li], w_ffn_up[li], w_ffn_down[li])
        else:
            h = h + hyena_mixer(hn, w_hy_in[li], b_hy_in[li], w_hy_conv[li], b_hy_conv[li],
                                w_hy_f1[li], b_hy_f1[li], w_hy_f2[li], b_hy_f2[li], hy_freq[li],
                                w_hy_f3[li], hy_bias[li], w_hy_out[li])
            hn2 = rmsnorm(h, ln_ffn[i]).reshape(B * S, D)
            h = h + moe_swiglu(hn2, w_router[li], w_exp_gate[li], w_exp_up[li],
                               w_exp_down[li]).reshape(B, S, D)
        gate = jax.nn.sigmoid(rmsnorm(h, ln_ple[i]) @ w_ple_gate[i])
        h = h + gate * (p[i] @ w_ple_proj[i])
    return rmsnorm(h, final_norm)
```

```python
import numpy as np
from contextlib import ExitStack
import concourse.bass as bass
import concourse.mybir as mybir
from concourse.bass_utils import run_bass_kernel_spmd

F32 = mybir.dt.float32
BF16 = mybir.dt.bfloat16
AF = mybir.ActivationFunctionType
ALU = mybir.AluOpType

ENGS = ['pe', 'act', 'dve', 'pool', 'sp']
NCORES = 8
D = 2048
KC = 16
S = 4096
B = 2
NEG = -30000.0
EPS = 1e-6


class Res:
    __slots__ = ('w', 'r')

    def __init__(self):
        self.w = None
        self.r = {}


class Prog:
    def __init__(self, nc, es, n_dma_sems=40):
        self.nc = nc
        self.es = es
        self.streams = {e: [] for e in ENGS}
        self.sems = {}
        for e in ENGS:
            self.sems[e] = es.enter_context(nc.semaphore('c_' + e))
        self.ecnt = {e: 0 for e in ENGS}
        self.nd = n_dma_sems
        for i in range(n_dma_sems):
            self.sems[('d', i)] = es.enter_context(nc.semaphore('d%d' % i))
        self.dcnt = [0] * n_dma_sems
        self.dnext = 0
        self.known = {e: {} for e in ENGS}
        self.pbanks = []
        self.pnext = 0
        self.uid = 0

    def sbuf(self, shape, dtype=F32, name=None):
        self.uid += 1
        return self.es.enter_context(self.nc.sbuf_tensor(name or ('t%d' % self.uid), list(shape), dtype))

    def psum(self, shape, dtype=F32, name=None):
        self.uid += 1
        return self.es.enter_context(self.nc.psum_tensor(name or ('p%d' % self.uid), list(shape), dtype))

    def make_psum_pool(self, n):
        self.pbanks = [(self.psum([128, 512]), Res()) for _ in range(n)]

    def next_psum(self):
        b = self.pbanks[self.pnext]
        self.pnext = (self.pnext + 1) % len(self.pbanks)
        return b

    def _need(self, eng, key, val):
        if key == eng and eng in ('pe', 'sp'):
            return
        if self.known[eng].get(key, 0) >= val:
            return
        self.known[eng][key] = val
        self.streams[eng].append(('wait', key, val))

    def _deps(self, eng, reads, writes):
        for r in reads:
            if r.w is not None:
                self._need(eng, r.w[0], r.w[1])
        for w in writes:
            if w.w is not None:
                self._need(eng, w.w[0], w.w[1])
            for k, v in w.r.items():
                self._need(eng, k, v)

    def op(self, eng, name, args, kwargs=None, reads=(), writes=()):
        self._deps(eng, reads, writes)
        self.ecnt[eng] += 1
        c = self.ecnt[eng]
        self.streams[eng].append(('op', name, args, kwargs or {}, eng, 1))
        for r in reads:
            r.r[eng] = c
        for w in writes:
            w.w = (eng, c)
            w.r = {}

    def dma(self, q, out, in_, reads=(), writes=()):
        s = self.dnext
        self.dnext = (s + 1) % self.nd
        key = ('d', s)
        if self.dcnt[s] > 0:
            self._need(q, key, self.dcnt[s])
        self._deps(q, reads, writes)
        self.dcnt[s] += 16
        v = self.dcnt[s]
        self.streams[q].append(('op', 'dma_start', (), {'out': out, 'in_': in_}, key, 16))
        for r in reads:
            r.r[key] = v
        for w in writes:
            w.w = (key, v)
            w.r = {}

    def barrier(self):
        keys = [(e, self.ecnt[e]) for e in ENGS if self.ecnt[e] > 0]
        keys += [(('d', s), self.dcnt[s]) for s in range(self.nd) if self.dcnt[s] > 0]
        for e in ENGS:
            for k, v in keys:
                self._need(e, k, v)

    def finish(self):
        for s in range(self.nd):
            if self.dcnt[s] > 0:
                self._need('sp', ('d', s), self.dcnt[s])

    def emit(self):
        nc = self.nc
        sems = self.sems
        streams = self.streams

        def replay(name, eng):
            for it in streams[name]:
                if it[0] == 'wait':
                    eng.wait_ge(sems[it[1]], it[2])
                else:
                    ins = getattr(eng, it[1])(*it[2], **it[3])
                    ins.then_inc(sems[it[4]], it[5])

        with nc.Block() as block:
            @block.tensor
            def _(e):
                replay('pe', e)

            @block.scalar
            def _(e):
                replay('act', e)

            @block.vector
            def _(e):
                replay('dve', e)

            @block.gpsimd
            def _(e):
                replay('pool', e)

            @block.sync
            def _(e):
                replay('sp', e)

    def mm(self, out, lhsT, rhs, start, stop, reads, writes):
        self.op('pe', 'matmul', (out, lhsT, rhs), {'start': start, 'stop': stop}, reads, writes)

    def act(self, out, in_, func, reads, writes, **kw):
        kw = dict(kw)
        kw.update({'out': out, 'in_': in_, 'func': func})
        self.op('act', 'activation', (), kw, reads, writes)

    def tt(self, out, in0, in1, op, reads, writes, eng='dve'):
        self.op(eng, 'tensor_tensor', (out, in0, in1, op), {}, reads, writes)

    def ts(self, out, in0, s1, s2, op0, op1, reads, writes, eng='dve'):
        self.op(eng, 'tensor_scalar', (out, in0, s1, s2, op0, op1), {}, reads, writes)

    def stt(self, out, in0, scalar, in1, op0, op1, reads, writes):
        self.op('dve', 'scalar_tensor_tensor', (out, in0, scalar, in1, op0, op1), {}, reads, writes)

    def copy(self, out, in_, reads, writes, eng='dve'):
        self.op(eng, 'tensor_copy', (out, in_), {}, reads, writes)

    def recip(self, out, in_, reads, writes):
        self.op('dve', 'reciprocal', (out, in_), {}, reads, writes)

    def memset(self, ap, val, writes, eng='dve'):
        self.op(eng, 'memset', (ap, val), {}, (), writes)


class Rot:
    def __init__(self, P, n, shape, dtype, nres=1):
        self.bufs = [(P.sbuf(shape, dtype), [Res() for _ in range(nres)]) for _ in range(n)]
        self.i = 0

    def next(self):
        b = self.bufs[self.i]
        self.i = (self.i + 1) % len(self.bufs)
        return b


class Ctx:
    def __init__(self, P, T, ffn=False):
        self.P = P
        self.T = T
        self.NTB = T // 512
        P.make_psum_pool(7)
        self.ones = P.sbuf([128, 128], BF16)
        self.ones_r = Res()
        P.memset(self.ones[:], 1.0, [self.ones_r])
        self.sq = Rot(P, 3, [128, 512], BF16)
        self.t32 = Rot(P, 4, [128, 512], F32)
        self.w256 = Rot(P, 4, [128, KC, 256], BF16, nres=4)
        if ffn:
            self.wd = Rot(P, 2, [128, 2, 2048], BF16, nres=2)
            self.abuf = Rot(P, 2, [128, 2, T], BF16, nres=2)

    def load_w256(self, W, c0, kc_n=KC):
        P = self.P
        t, rs = self.w256.next()
        step = 4
        for i, k0 in enumerate(range(0, kc_n, step)):
            k1 = min(kc_n, k0 + step)
            P.dma('pool', t[:, k0:k1, :],
                  W[k0 * 128:k1 * 128, c0:c0 + 256].rearrange("(kc p) f -> p kc f", p=128),
                  writes=[rs[i]])
        return t, rs


def load_fm(P, dst, dram, nk, T, res_list, q='pool', step=4):
    for k0 in range(0, nk, step):
        k1 = min(nk, k0 + step)
        P.dma(q, dst[:, k0:k1, :], dram[k0 * 128:k1 * 128, :].rearrange("(kc p) t -> p kc t", p=128),
              writes=res_list[k0:k1])


def gemm_fm(C, W, kc_n, F, x, x_res, epilogue):
    P = C.P
    for blk in range(F // 256):
        wt, wr = C.load_w256(W, blk * 256, kc_n)
        for oo in range(2):
            o = blk * 2 + oo
            for tb in range(C.NTB):
                ps, pr = P.next_psum()
                for kc in range(kc_n):
                    P.mm(ps[:], wt[:, kc, oo * 128:(oo + 1) * 128], x[:, kc, tb * 512:(tb + 1) * 512],
                         kc == 0, kc == kc_n - 1, reads=[wr[kc // 4], x_res[kc]], writes=[pr])
                epilogue(o, tb, ps, pr)


def rmsnorm_fm(C, h, h_res, g_sb, g_res, out, out_res, out32=None, out32_res=None, after_tb=None):
    P = C.P
    for tb in range(C.NTB):
        sl = slice(tb * 512, (tb + 1) * 512)
        ps, pr = P.next_psum()
        for c in range(KC):
            sq, sr = C.sq.next()
            P.act(sq[:], h[:, c, sl], AF.Square, reads=[h_res[c]], writes=[sr[0]])
            P.mm(ps[:], C.ones[:], sq[:], c == 0, c == KC - 1, reads=[sr[0], C.ones_r], writes=[pr])
        v, vr = C.t32.next()
        P.ts(v[:], ps[:], 1.0 / D, EPS, ALU.mult, ALU.add, reads=[pr], writes=[vr[0]])
        sd, sdr = C.t32.next()
        P.act(sd[:], v[:], AF.Sqrt, reads=[vr[0]], writes=[sdr[0]])
        rs, rsr = C.t32.next()
        P.recip(rs[:], sd[:], reads=[sdr[0]], writes=[rsr[0]])
        for c in range(KC):
            if out32 is None:
                P.stt(out[:, c, sl], h[:, c, sl], g_sb[:, c:c + 1], rs[:], ALU.mult, ALU.mult,
                      reads=[h_res[c], g_res, rsr[0]], writes=[out_res[c]])
            else:
                P.stt(out32[:, c, :], h[:, c, sl], g_sb[:, c:c + 1], rs[:], ALU.mult, ALU.mult,
                      reads=[h_res[c], g_res, rsr[0]], writes=[out32_res[c]])
                P.act(out[:, c, sl], out32[:, c, :], AF.Copy, reads=[out32_res[c]], writes=[out_res[c]])
        if after_tb is not None:
            after_tb(tb)


def swiglu_fm(C, Wg, Wu, Wd, FF, xn, xn_res, h, h_res, gate=None):
    P = C.P
    for grp in range(FF // 256):
        wg, wgr = C.load_w256(Wg, grp * 256)
        wu, wur = C.load_w256(Wu, grp * 256)
        wd, wdr = C.wd.next()
        for cc in range(2):
            r0 = grp * 256 + cc * 128
            P.dma('pool', wd[:, cc, :], Wd[r0:r0 + 128, :], writes=[wdr[cc]])
        a, ar = C.abuf.next()
        for cc in range(2):
            for tb in range(C.NTB):
                sl = slice(tb * 512, (tb + 1) * 512)
                psg, pgr = P.next_psum()
                for kc in range(KC):
                    P.mm(psg[:], wg[:, kc, cc * 128:(cc + 1) * 128], xn[:, kc, sl], kc == 0, kc == KC - 1,
                         reads=[wgr[kc // 4], xn_res[kc]], writes=[pgr])
                psu, pur = P.next_psum()
                for kc in range(KC):
                    P.mm(psu[:], wu[:, kc, cc * 128:(cc + 1) * 128], xn[:, kc, sl], kc == 0, kc == KC - 1,
                         reads=[wur[kc // 4], xn_res[kc]], writes=[pur])
                s, sr = C.t32.next()
                P.act(s[:], psg[:], AF.Silu, reads=[pgr], writes=[sr[0]])
                if gate is None:
                    P.tt(a[:, cc, sl], s[:], psu[:], ALU.mult, reads=[sr[0], pur], writes=[ar[cc]])
                else:
                    t, tr = C.t32.next()
                    P.tt(t[:], s[:], psu[:], ALU.mult, reads=[sr[0], pur], writes=[tr[0]])
                    P.tt(a[:, cc, sl], t[:], gate[0][:, sl], ALU.mult, reads=[tr[0], gate[1]], writes=[ar[cc]])
        for o in range(KC):
            for tb in range(C.NTB):
                sl = slice(tb * 512, (tb + 1) * 512)
                ps, pr = P.next_psum()
                for cc in range(2):
                    P.mm(ps[:], wd[:, cc, o * 128:(o + 1) * 128], a[:, cc, sl], cc == 0, cc == 1,
                         reads=[wdr[cc], ar[cc]], writes=[pr])
                P.tt(h[:, o, sl], ps[:], h[:, o, sl], ALU.add, reads=[pr, h_res[o]], writes=[h_res[o]])


def ple_fm(C, Wgate, Wproj, xn, xn_res, pT, pT_res, h, h_res):
    P = C.P
    for blk in range(D // 256):
        wt, wr = C.load_w256(Wgate, blk * 256)
        wp, wpr = C.load_w256(Wproj, blk * 256, 2)
        for oo in range(2):
            o = blk * 2 + oo
            for tb in range(C.NTB):
                sl = slice(tb * 512, (tb + 1) * 512)
                ps, pr = P.next_psum()
                for kc in range(KC):
                    P.mm(ps[:], wt[:, kc, oo * 128:(oo + 1) * 128], xn[:, kc, sl], kc == 0, kc == KC - 1,
                         reads=[wr[kc // 4], xn_res[kc]], writes=[pr])
                ps2, pr2 = P.next_psum()
                for kc in range(2):
                    P.mm(ps2[:], wp[:, kc, oo * 128:(oo + 1) * 128], pT[:, kc, sl], kc == 0, kc == 1,
                         reads=[wpr[0], pT_res[kc]], writes=[pr2])
                s, sr = C.t32.next()
                P.act(s[:], ps[:], AF.Sigmoid, reads=[pr], writes=[sr[0]])
                t, tr = C.t32.next()
                P.tt(t[:], s[:], ps2[:], ALU.mult, reads=[sr[0], pr2], writes=[tr[0]])
                P.tt(h[:, o, sl], t[:], h[:, o, sl], ALU.add, reads=[tr[0], h_res[o]], writes=[h_res[o]])


def store_fm(P, dram, src, nk, res_list, q='sp'):
    for k in range(nk):
        P.dma(q, dram[k * 128:(k + 1) * 128, :], src[:, k, :], reads=[res_list[k]])


def new_nc():
    nc = bass.Bass("TRN2", target_bir_lowering=False)
    return nc


def din(nc, name, shape, dt=F32):
    return nc.dram_tensor(name, list(shape), dt, kind="ExternalInput").ap()


def dout(nc, name, shape, dt=F32):
    return nc.dram_tensor(name, list(shape), dt, kind="ExternalOutput").ap()


def build_A(T, F, with_bias):
    nc = new_nc()
    xT = din(nc, "xT", [D, T])
    g = din(nc, "g", [128, KC])
    W = din(nc, "W", [D, F])
    if with_bias:
        bia = din(nc, "bias", [128, F // 128])
    yT = dout(nc, "yT", [F, T])
    with ExitStack() as es:
        P = Prog(nc, es)
        C = Ctx(P, T)
        h = P.sbuf([128, KC, T], F32)
        h_res = [Res() for _ in range(KC)]
        load_fm(P, h, xT, KC, T, h_res, q='sp')
        g_sb = P.sbuf([128, KC], F32)
        g_res = Res()
        P.dma('sp', g_sb[:], g, writes=[g_res])
        if with_bias:
            b_sb = P.sbuf([128, F // 128], F32)
            b_res = Res()
            P.dma('sp', b_sb[:], bia, writes=[b_res])
        xn = P.sbuf([128, KC, T], BF16)
        xn_res = [Res() for _ in range(KC)]
        rmsnorm_fm(C, h, h_res, g_sb, g_res, xn, xn_res)
        ybuf = Rot(P, 4, [128, 512], F32)

        def epi(o, tb, ps, pr):
            y, yr = ybuf.next()
            if with_bias:
                P.act(y[:], ps[:], AF.Identity, reads=[pr, b_res], writes=[yr[0]], bias=b_sb[:, o:o + 1])
            else:
                if (o + tb) % 2 == 0:
                    P.copy(y[:], ps[:], reads=[pr], writes=[yr[0]])
                else:
                    P.act(y[:], ps[:], AF.Copy, reads=[pr], writes=[yr[0]])
            P.dma('sp', yT[o * 128:(o + 1) * 128, tb * 512:(tb + 1) * 512], y[:], reads=[yr[0]])

        gemm_fm(C, W, KC, F, xn, xn_res, epi)
        P.finish()
        P.emit()
    return nc


def build_B():
    nc = new_nc()
    qa_d = din(nc, "qa", [256, S])
    ka_d = din(nc, "ka", [128, S])
    va_d = din(nc, "va", [S, 128])
    qn_d = din(nc, "qn", [256, S])
    kn_d = din(nc, "kn", [256, S])
    vn_d = din(nc, "vn", [S, 256])
    wb_d = din(nc, "wbias", [128, 4 * 384])
    nb_d = din(nc, "nbias", [128, 4 * 5 * 640])
    sk_d = din(nc, "sink", [128, 4])
    mix_d = dout(nc, "mixT", [512, S])
    NT = S // 128
    with ExitStack() as es:
        P = Prog(nc, es)
        ones = P.sbuf([128, 128], BF16)
        ones_r = Res()
        P.memset(ones[:], 1.0, [ones_r])
        qa = P.sbuf([128, 2, S], BF16); qa_r = [Res() for _ in range(2)]
        ka = P.sbuf([128, 1, S], BF16); ka_r = [Res()]
        qn = P.sbuf([128, 2, S], BF16); qn_r = [Res() for _ in range(2)]
        kn = P.sbuf([128, 2, S], BF16); kn_r = [Res() for _ in range(2)]
        va = P.sbuf([128, NT, 128], BF16); va_r = Res()
        vn = P.sbuf([128, NT, 256], BF16); vn_r = Res()
        wb = P.sbuf([128, 4, 384], F32); wb_r = Res()
        nb = P.sbuf([128, 20, 640], F32); nb_r = Res()
        sk = P.sbuf([128, 4], F32); sk_r = Res()
        esk = P.sbuf([128, 4], F32); esk_r = Res()
        load_fm(P, qa, qa_d, 2, S, qa_r, step=1)
        load_fm(P, ka, ka_d, 1, S, ka_r, step=1)
        load_fm(P, qn, qn_d, 2, S, qn_r, step=1)
        load_fm(P, kn, kn_d, 2, S, kn_r, step=1)
        for t0 in range(0, NT, 8):
            P.dma('pool', va[:, t0:t0 + 8, :], va_d[t0 * 128:(t0 + 8) * 128, :].rearrange("(t p) c -> p t c", p=128), writes=[va_r])
            P.dma('pool', vn[:, t0:t0 + 8, :], vn_d[t0 * 128:(t0 + 8) * 128, :].rearrange("(t p) c -> p t c", p=128), writes=[vn_r])
        P.dma('sp', wb[:], wb_d.rearrange("p (j c) -> p j c", j=4), writes=[wb_r])
        for j in range(4):
            P.dma('sp', nb[:, j * 5:(j + 1) * 5, :], nb_d[:, j * 3200:(j + 1) * 3200].rearrange("p (a c) -> p a c", a=5), writes=[nb_r])
        P.dma('sp', sk[:], sk_d, writes=[sk_r])
        P.barrier()
        P.act(esk[:], sk[:], AF.Exp, reads=[sk_r], writes=[esk_r])

        ps_s = [(P.psum([128, 1024]), Res()) for _ in range(2)]
        ps_o = [(P.psum([128, 512]), Res()) for _ in range(2)]
        ps_d = [(P.psum([128, 512]), Res()) for _ in range(2)]
        tbuf = Rot(P, 3, [128, 640], F32)
        pbuf = Rot(P, 3, [128, 640], BF16)
        dbuf = Rot(P, 3, [128, 128], F32)
        obuf = Rot(P, 4, [128, 128], F32)
        it = 0

        def attend(q, q_r, k, k_r, v, v_r, vcol, c, pb, qi, ktiles, bias_ap, bias_r, esink, esink_r, out_rows):
            nonlocal it
            n = len(ktiles)
            pss, pssr = ps_s[it % 2]
            pso, psor = ps_o[it % 2]
            psd, psdr = ps_d[it % 2]
            it += 1
            for i, kt in enumerate(ktiles):
                P.mm(pss[:, i * 128:(i + 1) * 128], k[pb:pb + 64, c if k is not ka else 0, kt * 128:(kt + 1) * 128],
                     q[pb:pb + 64, c, qi * 128:(qi + 1) * 128], True, True, reads=[k_r, q_r], writes=[pssr])
            t, tr = tbuf.next()
            w = n * 128
            for c0 in range(0, w, 512):
                c1 = min(w, c0 + 512)
                P.stt(t[:, c0:c1], pss[:, c0:c1], 0.125, bias_ap[:, c0:c1], ALU.mult, ALU.add,
                      reads=[pssr, bias_r], writes=[tr[0]])
            p, pr = pbuf.next()
            P.act(p[:, 0:w], t[:, 0:w], AF.Exp, reads=[tr[0]], writes=[pr[0]])
            for i, kt in enumerate(ktiles):
                P.mm(pso[:, 0:128], v[:, kt, vcol:vcol + 128], p[:, i * 128:(i + 1) * 128], i == 0, i == n - 1,
                     reads=[v_r, pr[0]], writes=[psor])
            for i, kt in enumerate(ktiles):
                P.mm(psd[:, 0:128], ones[:], p[:, i * 128:(i + 1) * 128], i == 0, i == n - 1,
                     reads=[ones_r, pr[0]], writes=[psdr])
            dd, ddr = dbuf.next()
            if esink is not None:
                P.ts(dd[:], psd[:, 0:128], esink, None, ALU.add, ALU.bypass, reads=[psdr, esink_r], writes=[ddr[0]])
            else:
                P.copy(dd[:], psd[:, 0:128], reads=[psdr], writes=[ddr[0]])
            rd, rdr = dbuf.next()
            P.recip(rd[:], dd[:], reads=[ddr[0]], writes=[rdr[0]])
            o, orr = obuf.next()
            P.tt(o[pb:pb + 64, :], pso[pb:pb + 64, 0:128], rd[pb:pb + 64, :], ALU.mult, reads=[psor, rdr[0]], writes=[orr[0]])
            P.dma('sp', mix_d[out_rows + pb:out_rows + pb + 64, qi * 128:(qi + 1) * 128], o[pb:pb + 64, :], reads=[orr[0]])

        for j in range(4):
            c = j // 2
            pb = (j % 2) * 64
            for qi in range(NT):
                jbs = [jb for jb in range(3) if 0 <= qi + jb - 1 < NT]
                kts = [qi + jb - 1 for jb in jbs]
                attend(qa, qa_r[c], ka, ka_r[0], va, va_r, 0, c, pb, qi, kts,
                       wb[:, j, jbs[0] * 128:(jbs[-1] + 1) * 128], wb_r, esk[:, j:j + 1], esk_r, c * 128)
        for j in range(4):
            c = j // 2
            pb = (j % 2) * 64
            for m in range(NT):
                kb = min(max(2 * m - 4, 0), 54)
                kt0 = kb // 2
                pat = 0 if m == 0 else 1 if m == 1 else 3 if m == 30 else 4 if m == 31 else 2
                attend(qn, qn_r[c], kn, kn_r[c], vn, vn_r, c * 128, c, pb, m, [kt0 + i for i in range(5)],
                       nb[:, j * 5 + pat, :], nb_r, None, None, 256 + c * 128)
        P.finish()
        P.emit()
    return nc


def build_C(T):
    nc = new_nc()
    xT = din(nc, "xT", [D, T])
    mixT = din(nc, "mixT", [D, T])
    pT_d = din(nc, "pT", [256, T])
    g_ffn = din(nc, "g_ffn", [128, KC])
    g_ple = din(nc, "g_ple", [128, KC])
    Wo = din(nc, "w_out", [D, D])
    Wg = din(nc, "w_gate", [D, 5632])
    Wu = din(nc, "w_up", [D, 5632])
    Wd = din(nc, "w_down", [5632, D])
    Wpg = din(nc, "w_ple_gate", [D, D])
    Wpp = din(nc, "w_ple_proj", [256, D])
    hT_o = dout(nc, "hT", [D, T])
    with ExitStack() as es:
        P = Prog(nc, es)
        C = Ctx(P, T, ffn=True)
        h = P.sbuf([128, KC, T], F32)
        h_res = [Res() for _ in range(KC)]
        load_fm(P, h, xT, KC, T, h_res, q='sp')
        xn = P.sbuf([128, KC, T], BF16)
        xn_res = [Res() for _ in range(KC)]
        load_fm(P, xn, mixT, KC, T, xn_res, q='pool')
        pT = P.sbuf([128, 2, T], BF16)
        pT_res = [Res() for _ in range(2)]
        load_fm(P, pT, pT_d, 2, T, pT_res, q='pool', step=1)
        g1 = P.sbuf([128, KC], F32); g1r = Res()
        g2 = P.sbuf([128, KC], F32); g2r = Res()
        P.dma('sp', g1[:], g_ffn, writes=[g1r])
        P.dma('sp', g2[:], g_ple, writes=[g2r])

        def epi(o, tb, ps, pr):
            sl = slice(tb * 512, (tb + 1) * 512)
            P.tt(h[:, o, sl], ps[:], h[:, o, sl], ALU.add, reads=[pr, h_res[o]], writes=[h_res[o]])

        gemm_fm(C, Wo, KC, D, xn, xn_res, epi)
        rmsnorm_fm(C, h, h_res, g1, g1r, xn, xn_res)
        swiglu_fm(C, Wg, Wu, Wd, 5632, xn, xn_res, h, h_res)
        rmsnorm_fm(C, h, h_res, g2, g2r, xn, xn_res)
        ple_fm(C, Wpg, Wpp, xn, xn_res, pT, pT_res, h, h_res)
        store_fm(P, hT_o, h, KC, h_res)
        P.finish()
        P.emit()
    return nc


def fm_shards(a):
    F_ = a.shape[-1]
    flat = a.reshape(B * S, F_)
    return [np.ascontiguousarray(flat[i * 1024:(i + 1) * 1024].T) for i in range(NCORES)]


def from_fm_shards(lst):
    flat = np.concatenate([x.T for x in lst], axis=0)
    return np.ascontiguousarray(flat.reshape(B, S, -1))


def gvec(g):
    return np.ascontiguousarray(g.reshape(KC, 128).T)


def t5_bucket_np(rel):
    half = 16
    max_exact = 8
    n = np.abs(rel)
    log_ratio = np.log(np.maximum(n, 1).astype(np.float32) / max_exact) / np.float32(np.log(128 / max_exact))
    large = np.minimum(max_exact + (log_ratio * (half - max_exact)).astype(np.int32), half - 1)
    return np.where(rel > 0, half, 0) + np.where(n < max_exact, n, large)


def window_bias_tables(t5_bias):
    kk = np.arange(128)[:, None, None]
    jb = np.arange(3)[None, :, None]
    qq = np.arange(128)[None, None, :]
    rel = (jb - 1) * 128 + kk - qq
    ok = np.abs(rel) <= 128
    bucket = t5_bucket_np(rel)
    tab = t5_bias[bucket]
    tab = np.where(ok[..., None], tab, np.float32(NEG))
    return np.ascontiguousarray(np.transpose(tab, (3, 0, 1, 2))).astype(np.float32)


def na_bias_tables(rpb):
    out = np.empty((16, 5, 128, 5, 128), np.float32)
    for pi, m in enumerate([0, 1, 10, 30, 31]):
        kb = min(max(2 * m - 4, 0), 54)
        kk = np.arange(128)[:, None, None]
        tl = np.arange(5)[None, :, None]
        qq = np.arange(128)[None, None, :]
        r = 2 * m + qq // 64
        cq = qq % 64
        ka = tl * 128 + kk
        kr = kb + ka // 64
        ck = ka % 64
        rs = np.clip(r - 4, 0, 56)
        rok = (kr >= rs) & (kr < rs + 8)
        cs = np.clip(cq - 8, 0, 48)
        cok = (ck >= cs) & (ck < cs + 16)
        row_off = np.clip(kr - r + 7, 0, 14)
        col_off = np.clip(ck - cq, -15, 15) + 15
        row_off, col_off, ok = np.broadcast_arrays(row_off, col_off, rok & cok)
        tab = rpb[:, row_off, col_off]
        out[:, pi] = np.where(ok[None], tab, np.float32(NEG))
    return out


_NC_CACHE = {}


def get_nc(key, builder):
    if key not in _NC_CACHE:
        _NC_CACHE[key] = builder()
    return _NC_CACHE[key]


def run(nc, in_maps):
    res = run_bass_kernel_spmd(nc, in_maps, core_ids=list(range(NCORES)))
    return res.results


def layer0(x, p0, ln_mix, ln_ffn, ln_ple, t5_bias, w_attn_in, w_attn_out, attn_sink, na_rpb,
           w_ffn_gate, w_ffn_up, w_ffn_down, w_ple_proj, w_ple_gate):
    xs = fm_shards(x)
    ncA = get_nc('A0', lambda: build_A(1024, 4352, False))
    rA = run(ncA, [{"xT": xs[i], "g": gvec(ln_mix), "W": w_attn_in} for i in range(NCORES)])
    proj = from_fm_shards([r["yT"] for r in rA])
    wtab = window_bias_tables(t5_bias)
    ntab = na_bias_tables(na_rpb)
    in_maps = []
    for i in range(NCORES):
        b, hg = i // 4, i % 4
        g = hg // 2
        pr = proj[b]
        qa = pr[:, hg * 256:(hg + 1) * 256].T
        kg = pr[:, 1024 + g * 64:1024 + (g + 1) * 64]
        vg = pr[:, 1152 + g * 64:1152 + (g + 1) * 64]
        ka = np.concatenate([kg, kg], axis=1).T
        va = np.concatenate([vg, vg], axis=1)
        qn = pr[:, 1280 + hg * 256:1280 + (hg + 1) * 256].T
        kn = pr[:, 2304 + hg * 256:2304 + (hg + 1) * 256].T
        vn = pr[:, 3328 + hg * 256:3328 + (hg + 1) * 256]
        wbt = np.transpose(wtab[hg * 4:(hg + 1) * 4], (1, 0, 2, 3)).reshape(128, 4 * 384)
        nbt = np.transpose(ntab[hg * 4:(hg + 1) * 4], (2, 0, 1, 3, 4)).reshape(128, 4 * 5 * 640)
        sk = np.broadcast_to(attn_sink[hg * 4:(hg + 1) * 4][None, :], (128, 4))
        in_maps.append({k: np.ascontiguousarray(v, dtype=np.float32) for k, v in dict(
            qa=qa, ka=ka, va=va, qn=qn, kn=kn, vn=vn, wbias=wbt, nbias=nbt, sink=sk).items()})
    ncB = get_nc('B', build_B)
    rB = run(ncB, in_maps)
    mix = np.empty((B, S, 2048), np.float32)
    for i in range(NCORES):
        b, hg = i // 4, i % 4
        m = rB[i]["mixT"]
        mix[b, :, hg * 256:(hg + 1) * 256] = m[0:256].T
        mix[b, :, 1024 + hg * 256:1024 + (hg + 1) * 256] = m[256:512].T
    ms = fm_shards(mix)
    ps = fm_shards(p0)
    ncC = get_nc('C', lambda: build_C(1024))
    rC = run(ncC, [{"xT": xs[i], "mixT": ms[i], "pT": ps[i], "g_ffn": gvec(ln_ffn), "g_ple": gvec(ln_ple),
                    "w_out": w_attn_out, "w_gate": w_ffn_gate, "w_up": w_ffn_up, "w_down": w_ffn_down,
                    "w_ple_gate": w_ple_gate, "w_ple_proj": w_ple_proj} for i in range(NCORES)])
    h1 = from_fm_shards([r["hT"] for r in rC])
    return h1, proj, mix


NCH = 128
TWO_PI = 6.283185307179586
MAGIC = 12582912.0


def build_D():
    nc = new_nc()
    pp = din(nc, "pp", [B, S + 2, 3, 256])
    cw_d = din(nc, "cw", [128, 4 * 3 * 256])
    fb_d = din(nc, "fb", [128, 2 * 256])
    zT_d = din(nc, "zT", [33, S])
    fw1_d = din(nc, "fw1", [33, 64])
    fb1_d = din(nc, "fb1", [64, 1])
    fw2_d = din(nc, "fw2", [64, 64])
    fb2_d = din(nc, "fb2", [64, 1])
    frq_d = din(nc, "frq", [64, 1])
    fw3_d = din(nc, "fw3", [64, 4 * 256])
    ntn_d = din(nc, "ntn", [128, 32])
    adl_d = din(nc, "adl", [128, 256])
    Ctf = din(nc, "Ctf", [32, 128, 32 * 128], BF16)
    Stf = din(nc, "Stf", [32, 128, 32 * 128], BF16)
    Cft = din(nc, "Cft", [32, 128, 32 * 128], BF16)
    Sft = din(nc, "Sft", [32, 128, 32 * 128], BF16)
    out_d = dout(nc, "zf2", [B, S, 256])
    NT = 32
    N = 2 * NCH
    with ExitStack() as es:
        P = Prog(nc, es)
        P.make_psum_pool(8)
        cw = P.sbuf([128, 4, 3, 256], F32); cw_r = Res()
        fb = P.sbuf([128, 2, 256], F32); fb_r = Res()
        fw1 = P.sbuf([33, 64], F32); fw2 = P.sbuf([64, 64], F32); fw3 = P.sbuf([64, 1024], F32)
        fb1 = P.sbuf([64, 1], F32); fb2 = P.sbuf([64, 1], F32); frq = P.sbuf([64, 1], F32)
        ntn = P.sbuf([128, 32], F32); adl = P.sbuf([128, 256], F32)
        cr = Res()
        P.dma('sp', cw[:], cw_d.rearrange("p (k j c) -> p k j c", k=4, j=3), writes=[cw_r])
        P.dma('sp', fb[:], fb_d.rearrange("p (o c) -> p o c", o=2), writes=[fb_r])
        for t, d_ in ((fw1, fw1_d), (fw2, fw2_d), (fw3, fw3_d), (fb1, fb1_d), (fb2, fb2_d), (frq, frq_d),
                      (ntn, ntn_d), (adl, adl_d)):
            P.dma('sp', t[:], d_, writes=[cr])
        P.barrier()

        big = P.sbuf([128, 64, N], BF16)
        Hc = P.sbuf([128, NT, N], BF16)
        Hs = P.sbuf([128, NT, N], BF16)
        zb = P.sbuf([128, NT, N], BF16)
        dbuf = Rot(P, 4, [128, 32, 128], BF16)
        ld3 = Rot(P, 6, [128, 3, NCH], F32)
        sct = Rot(P, 3, [128, 3, NCH], F32)
        tmpA = Rot(P, 4, [128, 3, NCH], F32)
        t256 = Rot(P, 6, [128, N], F32)
        t64 = Rot(P, 6, [64, 512], F32)
        zblk = Rot(P, 2, [33, 512], F32)
        obuf = Rot(P, 3, [128, NCH], F32)

        def short_conv(tt, b, c0):
            lds = []
            for s_ in range(3):
                t, r = ld3.next()
                P.dma('sp', t[:], pp[b, tt * 128 + s_:tt * 128 + s_ + 128, :, c0:c0 + NCH], writes=[r[0]])
                lds.append((t, r[0]))
            acc, accr = sct.next()
            u, ur = tmpA.next()
            P.tt(acc[:], lds[0][0][:], cw[:, 0, :, c0:c0 + NCH], ALU.mult, reads=[lds[0][1], cw_r], writes=[accr[0]])
            P.tt(u[:], lds[1][0][:], cw[:, 1, :, c0:c0 + NCH], ALU.mult, reads=[lds[1][1], cw_r], writes=[ur[0]])
            P.tt(acc[:], acc[:], u[:], ALU.add, reads=[accr[0], ur[0]], writes=[accr[0]])
            u2, ur2 = tmpA.next()
            P.tt(u2[:], lds[2][0][:], cw[:, 2, :, c0:c0 + NCH], ALU.mult, reads=[lds[2][1], cw_r], writes=[ur2[0]])
            P.tt(acc[:], acc[:], u2[:], ALU.add, reads=[accr[0], ur2[0]], writes=[accr[0]])
            P.tt(acc[:], acc[:], cw[:, 3, :, c0:c0 + NCH], ALU.add, reads=[accr[0], cw_r], writes=[accr[0]])
            return acc, accr[0]

        def range_sin(ps, pr, bias, out, out_r):
            a, ar = t64.next()
            P.ts(a[:], ps[0:64, :], bias[:, 0:1], frq[:, 0:1], ALU.add, ALU.mult, reads=[pr], writes=[ar[0]])
            k, kr = t64.next()
            P.ts(k[:], a[:], 1.0 / TWO_PI, MAGIC, ALU.mult, ALU.add, reads=[ar[0]], writes=[kr[0]])
            k2, k2r = t64.next()
            P.ts(k2[:], k[:], MAGIC, -TWO_PI, ALU.subtract, ALU.mult, reads=[kr[0]], writes=[k2r[0]])
            P.tt(a[:], a[:], k2[:], ALU.add, reads=[ar[0], k2r[0]], writes=[ar[0]])
            P.ts(a[:], a[:], 3.1415925, -3.1415925, ALU.min, ALU.max, reads=[ar[0]], writes=[ar[0]])
            P.act(out[:], a[:], AF.Sin, reads=[ar[0]], writes=[out_r])

        def load_dft(M, idx):
            t, r = dbuf.next()
            P.dma('sp', t[:], M[idx].rearrange("p (c k) -> p c k", c=32), writes=[r[0]])
            return t, r[0]

        for cp in range(2):
            c0 = cp * NCH
            P.barrier()
            hs_r = [Res() for _ in range(NT)]
            hd_r = [Res() for _ in range(NT)]
            for tb in range(8):
                zt, ztr = zblk.next()
                P.dma('sp', zt[:], zT_d[:, tb * 512:(tb + 1) * 512], writes=[ztr[0]])
                ps, pr = P.next_psum()
                P.mm(ps[0:64, :], fw1[:], zt[:], True, True, reads=[ztr[0]], writes=[pr])
                h1, h1r = t64.next()
                range_sin(ps, pr, fb1, h1, h1r[0])
                ps2, pr2 = P.next_psum()
                P.mm(ps2[0:64, :], fw2[:], h1[:], True, True, reads=[h1r[0]], writes=[pr2])
                h2, h2r = t64.next()
                range_sin(ps2, pr2, fb2, h2, h2r[0])
                for tq in range(4):
                    tt = tb * 4 + tq
                    psf, pfr = P.next_psum()
                    for q in range(4):
                        P.mm(psf[:, q * 128:(q + 1) * 128], h2[:, tq * 128:(tq + 1) * 128],
                             fw3[:, q * 256 + c0:q * 256 + c0 + NCH], True, True, reads=[h2r[0]], writes=[pfr])
                    dec, decr = t256.next()
                    for o in range(2):
                        P.act(dec[:, o * 128:(o + 1) * 128], adl[:, c0:c0 + NCH], AF.Exp, reads=[], writes=[decr[0]],
                              scale=ntn[:, tt:tt + 1])
                    sb, sbr = t256.next()
                    P.act(sb[:], psf[:, 256:512], AF.Copy, reads=[pfr], writes=[sbr[0]])
                    s1, s1r = t256.next()
                    P.tt(s1[:], psf[:, 0:256], sb[:], ALU.add, reads=[pfr, sbr[0]], writes=[s1r[0]])
                    P.tt(big[:, tt, :], s1[:], dec[:], ALU.mult, reads=[s1r[0], decr[0]], writes=[hs_r[tt]])
                    d1, d1r = t256.next()
                    P.tt(d1[:], psf[:, 0:256], sb[:], ALU.subtract, reads=[pfr, sbr[0]], writes=[d1r[0]])
                    P.tt(big[:, 32 + tt, :], d1[:], dec[:], ALU.mult, reads=[d1r[0], decr[0]], writes=[hd_r[tt]])
            zb_r = [Res() for _ in range(NT)]
            for tt in range(NT):
                for b in range(2):
                    sc, scr = short_conv(tt, b, c0)
                    P.copy(zb[:, tt, b * NCH:(b + 1) * NCH], sc[:, 2, :], reads=[scr], writes=[zb_r[tt]])
            H_r = Res()
            for kt in range(NT):
                ct, ctr = load_dft(Ctf, kt)
                st, str_ = load_dft(Stf, kt)
                ps, pr = P.next_psum()
                for tc in range(NT):
                    P.mm(ps[:, 0:N], ct[:, tc, :], big[:, tc, :], tc == 0, tc == NT - 1, reads=[ctr, hs_r[tc]], writes=[pr])
                ps2, pr2 = P.next_psum()
                for tc in range(NT):
                    P.mm(ps2[:, 0:N], st[:, tc, :], big[:, 32 + tc, :], tc == 0, tc == NT - 1, reads=[str_, hd_r[tc]], writes=[pr2])
                P.copy(Hc[:, kt, :], ps[:, 0:N], reads=[pr], writes=[H_r])
                P.act(Hs[:, kt, :], ps2[:, 0:N], AF.Copy, reads=[pr2], writes=[H_r])
            P.barrier()

            def forward(o):
                y_r = [Res() for _ in range(64)]
                for kt in range(NT):
                    ct, ctr = load_dft(Ctf, kt)
                    st, str_ = load_dft(Stf, kt)
                    ps, pr = P.next_psum()
                    for tc in range(NT):
                        P.mm(ps[:, 0:N], ct[:, tc, :], zb[:, tc, :], tc == 0, tc == NT - 1, reads=[ctr, zb_r[tc]], writes=[pr])
                    ps2, pr2 = P.next_psum()
                    for tc in range(NT):
                        P.mm(ps2[:, 0:N], st[:, tc, :], zb[:, tc, :], tc == 0, tc == NT - 1, reads=[str_, zb_r[tc]], writes=[pr2])
                    for b in range(2):
                        bs = slice(b * NCH, (b + 1) * NCH)
                        hsl = slice(o * NCH, (o + 1) * NCH)
                        t1, t1r = obuf.next()
                        P.tt(t1[:], ps[:, bs], Hc[:, kt, hsl], ALU.mult, reads=[pr, H_r], writes=[t1r[0]])
                        t2, t2r = obuf.next()
                        P.tt(t2[:], ps2[:, bs], Hs[:, kt, hsl], ALU.mult, reads=[pr2, H_r], writes=[t2r[0]])
                        P.tt(big[:, kt, bs], t1[:], t2[:], ALU.subtract, reads=[t1r[0], t2r[0]], writes=[y_r[kt]])
                        t3, t3r = obuf.next()
                        P.tt(t3[:], ps[:, bs], Hs[:, kt, hsl], ALU.mult, reads=[pr, H_r], writes=[t3r[0]])
                        t4, t4r = obuf.next()
                        P.tt(t4[:], ps2[:, bs], Hc[:, kt, hsl], ALU.mult, reads=[pr2, H_r], writes=[t4r[0]])
                        P.tt(big[:, 32 + kt, bs], t3[:], t4[:], ALU.add, reads=[t3r[0], t4r[0]], writes=[y_r[32 + kt]])
                return y_r

            def inverse(y_r, o, last):
                for tt in range(NT):
                    ct, ctr = load_dft(Cft, tt)
                    st, str_ = load_dft(Sft, tt)
                    ps, pr = P.next_psum()
                    for kc in range(NT):
                        P.mm(ps[:, 0:N], ct[:, kc, :], big[:, kc, :], kc == 0, False, reads=[ctr, y_r[kc]], writes=[pr])
                    for kc in range(NT):
                        P.mm(ps[:, 0:N], st[:, kc, :], big[:, 32 + kc, :], False, kc == NT - 1, reads=[str_, y_r[32 + kc]], writes=[pr])
                    for b in range(2):
                        bs = slice(b * NCH, (b + 1) * NCH)
                        sc, scr = short_conv(tt, b, c0)
                        u, ur = obuf.next()
                        if not last:
                            P.tt(u[:], sc[:, 2, :], fb[:, 0, c0:c0 + NCH], ALU.mult, reads=[scr, fb_r], writes=[ur[0]])
                        else:
                            P.tt(u[:], zb[:, tt, bs], fb[:, 1, c0:c0 + NCH], ALU.mult, reads=[zb_r[tt], fb_r], writes=[ur[0]])
                        P.tt(u[:], u[:], ps[:, bs], ALU.add, reads=[ur[0], pr], writes=[ur[0]])
                        if not last:
                            P.tt(zb[:, tt, bs], u[:], sc[:, 0, :], ALU.mult, reads=[ur[0], scr], writes=[zb_r[tt]])
                        else:
                            ot, otr = obuf.next()
                            P.tt(ot[:], u[:], sc[:, 1, :], ALU.mult, reads=[ur[0], scr], writes=[otr[0]])
                            P.dma('sp', out_d[b, tt * 128:(tt + 1) * 128, c0:c0 + NCH], ot[:], reads=[otr[0]])

            y_r = forward(0)
            P.barrier()
            inverse(y_r, 0, False)
            P.barrier()
            y_r = forward(1)
            P.barrier()
            inverse(y_r, 1, True)
        P.finish()
        P.emit()
    return nc


_DFT_CACHE = {}


def dft_consts():
    if 'm' not in _DFT_CACHE:
        import ml_dtypes
        L = S
        Nn = 2 * L
        k = np.arange(L, dtype=np.float64)[:, None] + 0.5
        t = np.arange(L, dtype=np.float64)[None, :]
        ang = 2.0 * np.pi * k * t / Nn
        Cm = np.cos(ang)
        Sm = np.sin(ang)

        def tile_tf(M):
            Mt = M.T.reshape(32, 128, 32, 128)
            return np.ascontiguousarray(np.transpose(Mt, (2, 1, 0, 3))).reshape(32, 128, 32 * 128)

        def tile_ft(M):
            Mk = (M * (2.0 / Nn)).reshape(32, 128, 32, 128)
            return np.ascontiguousarray(np.transpose(Mk, (2, 1, 0, 3))).reshape(32, 128, 32 * 128)

        bf = ml_dtypes.bfloat16
        _DFT_CACHE['m'] = dict(Ctf=tile_tf(Cm).astype(np.float32).astype(bf), Stf=tile_tf(Sm).astype(np.float32).astype(bf),
                               Cft=tile_ft(Cm).astype(np.float32).astype(bf), Sft=tile_ft(Sm).astype(np.float32).astype(bf))
    return _DFT_CACHE['m']


def hyena_pos_features():
    L = S
    t = np.linspace(0.0, 1.0, L, dtype=np.float32)[:, None]
    bands = 16
    w = (np.float32(2.0 * np.pi / L)) * np.arange(L, dtype=np.float32)[:, None]
    f = np.linspace(1e-4, bands - 1, bands, dtype=np.float32)[None]
    z = np.concatenate([t, np.cos(f * w), -np.sin(f * w)], axis=-1).astype(np.float32)
    return z, t[:, 0]


def hyena_core(hyproj, w_hy_conv, b_hy_conv, fw1, fb1, fw2, fb2, freq, fw3, fbias):
    consts = dft_consts()
    z, tn = hyena_pos_features()
    zT = np.ascontiguousarray(z.T)
    ntn = np.ascontiguousarray((-tn).reshape(32, 128).T).astype(np.float32)
    import math
    max_decay = math.log(1e-2) / 0.3
    min_decay = math.log(1e-2) / 1.5
    deltas = np.abs(np.linspace(min_decay, max_decay, 2048, dtype=np.float32))
    pr4 = hyproj.reshape(B, S, 3, 2048)
    in_maps = []
    for i in range(NCORES):
        cs = slice(i * 256, (i + 1) * 256)
        pp = np.zeros((B, S + 2, 3, 256), np.float32)
        pp[:, 1:S + 1] = pr4[:, :, :, cs]
        cwk = np.stack([w_hy_conv[0].reshape(3, 2048)[:, cs], w_hy_conv[1].reshape(3, 2048)[:, cs],
                        w_hy_conv[2].reshape(3, 2048)[:, cs], b_hy_conv.reshape(3, 2048)[:, cs]], axis=0)
        cw = np.broadcast_to(cwk.reshape(1, -1), (128, 4 * 3 * 256))
        fb = np.broadcast_to(fbias[:, cs].reshape(1, -1), (128, 512))
        f3 = fw3.reshape(64, 2, 2, 2048)[:, :, :, cs].reshape(64, 1024)
        adl = np.broadcast_to(deltas[cs][None, :], (128, 256))
        m = dict(pp=pp, cw=cw, fb=fb, zT=zT, fw1=fw1, fb1=fb1.reshape(64, 1), fw2=fw2, fb2=fb2.reshape(64, 1),
                 frq=freq.reshape(64, 1), fw3=f3, ntn=ntn, adl=adl)
        m = {k: np.ascontiguousarray(v, dtype=np.float32) for k, v in m.items()}
        m.update(consts)
        in_maps.append(m)
    ncD = get_nc('D', build_D)
    rD = run(ncD, in_maps)
    return np.concatenate([r["zf2"] for r in rD], axis=-1)


def build_E1(T):
    nc = new_nc()
    hT_d = din(nc, "hT", [D, T])
    zT_d = din(nc, "zfT", [D, T])
    Wo = din(nc, "w_out", [D, D])
    g_d = din(nc, "g", [128, KC])
    wr_d = din(nc, "w_router", [128, KC * 8])
    ho_d = dout(nc, "hT_out", [D, T])
    hn_d = dout(nc, "hnT", [D, T])
    G_d = dout(nc, "G", [T, 8])
    with ExitStack() as es:
        P = Prog(nc, es)
        C = Ctx(P, T)
        h = P.sbuf([128, KC, T], F32)
        h_res = [Res() for _ in range(KC)]
        load_fm(P, h, hT_d, KC, T, h_res, q='sp')
        xn = P.sbuf([128, KC, T], BF16)
        xn_res = [Res() for _ in range(KC)]
        load_fm(P, xn, zT_d, KC, T, xn_res, q='pool')
        g1 = P.sbuf([128, KC], F32); g1r = Res()
        P.dma('sp', g1[:], g_d, writes=[g1r])
        wr = P.sbuf([128, KC, 8], F32); wrr = Res()
        P.dma('sp', wr[:], wr_d.rearrange("p (k e) -> p k e", k=KC), writes=[wrr])

        def epi(o, tb, ps, pr):
            sl = slice(tb * 512, (tb + 1) * 512)
            P.tt(h[:, o, sl], ps[:], h[:, o, sl], ALU.add, reads=[pr, h_res[o]], writes=[h_res[o]])

        gemm_fm(C, Wo, KC, D, xn, xn_res, epi)
        store_fm(P, ho_d, h, KC, h_res)
        hn32 = P.sbuf([128, KC, 512], F32)
        hn32_res = [Res() for _ in range(KC)]
        s8 = Rot(P, 12, [128, 8], F32)

        def after_tb(tb):
            for c in range(KC):
                P.dma('sp', hn_d[c * 128:(c + 1) * 128, tb * 512:(tb + 1) * 512], hn32[:, c, :], reads=[hn32_res[c]])
            for tq in range(4):
                ps, pr = P.next_psum()
                for kc in range(KC):
                    P.mm(ps[:, 0:8], hn32[:, kc, tq * 128:(tq + 1) * 128], wr[:, kc, :], kc == 0, kc == KC - 1,
                         reads=[hn32_res[kc], wrr], writes=[pr])
                lg, lgr = s8.next()
                P.copy(lg[:], ps[:, 0:8], reads=[pr], writes=[lgr[0]])
                top, topr = s8.next()
                P.op('dve', 'max', (top[:], lg[:]), {}, reads=[lgr[0]], writes=[topr[0]])
                sc, scr = s8.next()
                P.tt(sc[:, 0:1], top[:, 1:2], top[:, 0:1], ALU.subtract, reads=[topr[0]], writes=[scr[0]])
                P.act(sc[:, 1:2], sc[:, 0:1], AF.Exp, reads=[scr[0]], writes=[scr[0]])
                P.ts(sc[:, 2:3], sc[:, 1:2], 1.0, None, ALU.add, ALU.bypass, reads=[scr[0]], writes=[scr[0]])
                P.recip(sc[:, 3:4], sc[:, 2:3], reads=[scr[0]], writes=[scr[0]])
                P.tt(sc[:, 4:5], sc[:, 1:2], sc[:, 3:4], ALU.mult, reads=[scr[0]], writes=[scr[0]])
                m1, m1r = s8.next()
                P.ts(m1[:], lg[:], top[:, 0:1], sc[:, 3:4], ALU.is_equal, ALU.mult, reads=[lgr[0], topr[0], scr[0]], writes=[m1r[0]])
                m2, m2r = s8.next()
                P.ts(m2[:], lg[:], top[:, 1:2], sc[:, 4:5], ALU.is_equal, ALU.mult, reads=[lgr[0], topr[0], scr[0]], writes=[m2r[0]])
                gg, ggr = s8.next()
                P.tt(gg[:], m1[:], m2[:], ALU.add, reads=[m1r[0], m2r[0]], writes=[ggr[0]])
                r0 = tb * 512 + tq * 128
                P.dma('sp', G_d[r0:r0 + 128, :], gg[:], reads=[ggr[0]])

        rmsnorm_fm(C, h, h_res, g1, g1r, xn, xn_res, out32=hn32, out32_res=hn32_res, after_tb=after_tb)
        P.finish()
        P.emit()
    return nc


def build_E2(NTOK, FF):
    nc = new_nc()
    T = 1024
    xn_d = din(nc, "xnT", [D, NTOK])
    gt_d = din(nc, "gate", [128, NTOK])
    Wg = din(nc, "w_gate", [D, FF])
    Wu = din(nc, "w_up", [D, FF])
    Wd = din(nc, "w_down", [FF, D])
    y_d = dout(nc, "yT", [D, NTOK])
    with ExitStack() as es:
        P = Prog(nc, es)
        C = Ctx(P, T, ffn=True)
        xn = P.sbuf([128, KC, T], BF16)
        xn_res = [Res() for _ in range(KC)]
        acc = P.sbuf([128, KC, T], F32)
        acc_res = [Res() for _ in range(KC)]
        gt = P.sbuf([128, T], F32)
        gt_r = Res()
        for blk in range(NTOK // T):
            ts_ = slice(blk * T, (blk + 1) * T)
            load_fm(P, xn, xn_d[:, ts_], KC, T, xn_res, q='pool')
            P.dma('sp', gt[:], gt_d[:, ts_], writes=[gt_r])
            for c in range(KC):
                P.memset(acc[:, c, :], 0.0, [acc_res[c]])
            swiglu_fm(C, Wg, Wu, Wd, FF, xn, xn_res, acc, acc_res, gate=(gt, gt_r))
            for c in range(KC):
                P.dma('sp', y_d[c * 128:(c + 1) * 128, ts_], acc[:, c, :], reads=[acc_res[c]])
        P.finish()
        P.emit()
    return nc


def build_E3(T):
    nc = new_nc()
    hT_d = din(nc, "hT", [D, T])
    y_d = din(nc, "yT", [8, D, T])
    pT_d = din(nc, "pT", [256, T])
    g_ple = din(nc, "g_ple", [128, KC])
    g_fin = din(nc, "g_fin", [128, KC])
    Wpg = din(nc, "w_ple_gate", [D, D])
    Wpp = din(nc, "w_ple_proj", [256, D])
    o_d = dout(nc, "outT", [D, T])
    with ExitStack() as es:
        P = Prog(nc, es)
        C = Ctx(P, T)
        h = P.sbuf([128, KC, T], F32)
        h_res = [Res() for _ in range(KC)]
        load_fm(P, h, hT_d, KC, T, h_res, q='sp')
        xn = P.sbuf([128, KC, T], BF16)
        xn_res = [Res() for _ in range(KC)]
        pT = P.sbuf([128, 2, T], BF16)
        pT_res = [Res() for _ in range(2)]
        load_fm(P, pT, pT_d, 2, T, pT_res, q='pool', step=1)
        g2 = P.sbuf([128, KC], F32); g2r = Res()
        g3 = P.sbuf([128, KC], F32); g3r = Res()
        P.dma('sp', g2[:], g_ple, writes=[g2r])
        P.dma('sp', g3[:], g_fin, writes=[g3r])
        part = Rot(P, 3, [128, T], F32)
        for e in range(8):
            for k0 in range(KC):
                pt, ptr = part.next()
                P.dma('sp', pt[:], y_d[e, k0 * 128:(k0 + 1) * 128, :], writes=[ptr[0]])
                P.tt(h[:, k0, :], h[:, k0, :], pt[:], ALU.add, reads=[h_res[k0], ptr[0]], writes=[h_res[k0]])
        rmsnorm_fm(C, h, h_res, g2, g2r, xn, xn_res)
        ple_fm(C, Wpg, Wpp, xn, xn_res, pT, pT_res, h, h_res)
        o32 = P.sbuf([128, KC, 512], F32)
        o32_res = [Res() for _ in range(KC)]

        def after_tb(tb):
            for c in range(KC):
                P.dma('sp', o_d[c * 128:(c + 1) * 128, tb * 512:(tb + 1) * 512], o32[:, c, :], reads=[o32_res[c]])

        rmsnorm_fm(C, h, h_res, g3, g3r, xn, xn_res, out32=o32, out32_res=o32_res, after_tb=after_tb)
        P.finish()
        P.emit()
    return nc


def layer1(h1, p1, ln_mix, ln_ffn, ln_ple, final_norm, w_hy_in, b_hy_in, w_hy_conv, b_hy_conv, fw1, fb1, fw2, fb2,
           freq, fw3, fbias, w_hy_out, w_router, w_exp_gate, w_exp_up, w_exp_down, w_ple_proj, w_ple_gate, dbg=None):
    hs = fm_shards(h1)
    ncD0 = get_nc('D0', lambda: build_A(1024, 6144, True))
    bias = np.ascontiguousarray(b_hy_in.reshape(48, 128).T)
    r0 = run(ncD0, [{"xT": hs[i], "g": gvec(ln_mix), "W": w_hy_in, "bias": bias} for i in range(NCORES)])
    hyproj = from_fm_shards([r["yT"] for r in r0])
    zf2 = hyena_core(hyproj, w_hy_conv, b_hy_conv, fw1, fb1, fw2, fb2, freq, fw3, fbias)
    zs = fm_shards(zf2)
    wr = np.ascontiguousarray(np.transpose(w_router.reshape(KC, 128, 8), (1, 0, 2)).reshape(128, KC * 8))
    ncE1 = get_nc('E1', lambda: build_E1(1024))
    r1 = run(ncE1, [{"hT": hs[i], "zfT": zs[i], "w_out": w_hy_out, "g": gvec(ln_ffn), "w_router": wr}
                    for i in range(NCORES)])
    hh = [r["hT_out"] for r in r1]
    xn_all = np.ascontiguousarray(np.concatenate([r["hnT"] for r in r1], axis=1))
    G = np.concatenate([r["G"] for r in r1], axis=0)
    if dbg is not None:
        dbg.update(hyproj=hyproj, zf2=zf2, h_hy=from_fm_shards(hh), G=G, xn_all=xn_all)
    ncE2 = get_nc('E2', lambda: build_E2(B * S, 7168))
    r2 = run(ncE2, [{"xnT": xn_all, "gate": np.ascontiguousarray(np.broadcast_to(G[:, e][None, :], (128, B * S))),
                     "w_gate": w_exp_gate[e], "w_up": w_exp_up[e], "w_down": w_exp_down[e]} for e in range(NCORES)])
    ps = fm_shards(p1)
    ncE3 = get_nc('E3', lambda: build_E3(1024))
    in3 = []
    for i in range(NCORES):
        y = np.ascontiguousarray(np.stack([r2[e]["yT"][:, i * 1024:(i + 1) * 1024] for e in range(8)], axis=0))
        in3.append({"hT": hh[i], "yT": y, "pT": ps[i], "g_ple": gvec(ln_ple), "g_fin": gvec(final_norm),
                    "w_ple_gate": w_ple_gate, "w_ple_proj": w_ple_proj})
    r3 = run(ncE3, in3)
    return from_fm_shards([r["outT"] for r in r3])


def kernel(x, p, ln_mix, ln_ffn, ln_ple, final_norm, t5_bias, w_attn_in, w_attn_out, attn_sink, na_rpb,
           w_ffn_gate, w_ffn_up, w_ffn_down, w_hy_in, b_hy_in, w_hy_conv, b_hy_conv, w_hy_f1, b_hy_f1,
           w_hy_f2, b_hy_f2, hy_freq, w_hy_f3, hy_bias, w_hy_out, w_router, w_exp_gate, w_exp_up, w_exp_down,
           w_ple_proj, w_ple_gate):
    a = {k: np.asarray(v, dtype=np.float32) for k, v in locals().items()}
    h1, _, _ = layer0(a['x'], a['p'][0], a['ln_mix'][0], a['ln_ffn'][0], a['ln_ple'][0], a['t5_bias'],
                      a['w_attn_in'][0], a['w_attn_out'][0], a['attn_sink'][0], a['na_rpb'][0],
                      a['w_ffn_gate'][0], a['w_ffn_up'][0], a['w_ffn_down'][0], a['w_ple_proj'][0], a['w_ple_gate'][0])
    out = layer1(h1, a['p'][1], a['ln_mix'][1], a['ln_ffn'][1], a['ln_ple'][1], a['final_norm'],
                 a['w_hy_in'][0], a['b_hy_in'][0], a['w_hy_conv'][0], a['b_hy_conv'][0], a['w_hy_f1'][0], a['b_hy_f1'][0],
                 a['w_hy_f2'][0], a['b_hy_f2'][0], a['hy_freq'][0], a['w_hy_f3'][0], a['hy_bias'][0], a['w_hy_out'][0],
                 a['w_router'][0], a['w_exp_gate'][0], a['w_exp_up'][0], a['w_exp_down'][0],
                 a['w_ple_proj'][1], a['w_ple_gate'][1])
    return out.astype(np.float32)
```
